# Optimizing a Trainium2 kernel written in Bass

```python
import jax, jax.numpy as jnp
from jax import lax
import numpy as np


D_MODEL = 1024
BATCH = 2
SEQ = 8192
DEPTH = 1

N_HEADS_A = 8
N_HEADS_B = 8
HEAD_DIM = 64
ROPE_DIM = HEAD_DIM // 4
NOPE_DIM = HEAD_DIM - ROPE_DIM
KV_RANK = 128
IDX_HEADS = 8
IDX_DIM = 64
IDX_TOPK = 256
MOBA_BLOCK = 256
MOBA_TOPK = 3
Q_BLOCK = 128
D_FF = 2816
ROPE_THETA = 500000.0
EPS = 1e-6
N_MOD = 9
WIDTH_A = N_HEADS_A * HEAD_DIM
WIDTH_B = N_HEADS_B * HEAD_DIM
IN_SPLITS = (WIDTH_A, KV_RANK, ROPE_DIM, IDX_HEADS * IDX_DIM, IDX_DIM, IDX_HEADS, WIDTH_B, WIDTH_B, WIDTH_B, D_MODEL, D_MODEL)
IN_COLS = sum(IN_SPLITS)

kernel_name = 'hybrid_dsa_moba_macaron_adaln'


def rms_norm(x, g):
    xf = x.astype(jnp.float32)
    y = xf * lax.rsqrt(jnp.mean(xf * xf, axis=-1, keepdims=True) + EPS)
    return (y * g.astype(jnp.float32)).astype(x.dtype)


def modulate(h, shift, scale):
    return h * (1.0 + scale) + shift


def partial_rotary(x, pos):
    half = ROPE_DIM // 2
    inv_freq = jnp.power(ROPE_THETA, -jnp.arange(half, dtype=jnp.float32) / half)
    ang = pos.astype(jnp.float32)[..., None] * inv_freq
    if x.ndim == 4:
        ang = ang[:, :, None, :]
    cos, sin = jnp.cos(ang), jnp.sin(ang)
    xr = x[..., :ROPE_DIM].astype(jnp.float32)
    x1, x2 = xr[..., :half], xr[..., half:]
    rot = jnp.concatenate([x1 * cos - x2 * sin, x2 * cos + x1 * sin], axis=-1).astype(x.dtype)
    return jnp.concatenate([rot, x[..., ROPE_DIM:]], axis=-1)


def swiglu(h, w_in, w_out):
    a, b = jnp.split(h @ w_in, 2, axis=-1)
    return (jax.nn.silu(a) * b) @ w_out


def dsa_attention(q_a, ckv, k_rope, q_idx, k_idx, w_idx, w_uk, w_uv):
    B, S = q_a.shape[0], q_a.shape[1]
    top_k = min(IDX_TOPK, S // 4)
    key_pos = jnp.arange(S)
    k_idx32 = k_idx.astype(jnp.float32)
    b_ix = jnp.arange(B)[:, None, None]

    def block(i):
        start = i * Q_BLOCK
        q_pos = start + jnp.arange(Q_BLOCK)
        qi = lax.dynamic_slice_in_dim(q_idx, start, Q_BLOCK, axis=1).astype(jnp.float32)
        wi = lax.dynamic_slice_in_dim(w_idx, start, Q_BLOCK, axis=1).astype(jnp.float32)
        qa = lax.dynamic_slice_in_dim(q_a, start, Q_BLOCK, axis=1)
        logits = jnp.einsum('bqhd,bsd->bqhs', qi, k_idx32) * IDX_DIM ** -0.5
        score = jnp.einsum('bqh,bqhs->bqs', wi * IDX_HEADS ** -0.5, jax.nn.relu(logits))
        causal = key_pos[None, :] <= q_pos[:, None]
        score = jnp.where(causal[None], score, -jnp.inf)
        _, sel = lax.top_k(score, top_k)
        valid = sel <= q_pos[None, :, None]
        c_sel = ckv[b_ix, sel]
        r_sel = k_rope[b_ix, sel]
        q_lat = jnp.einsum('bqhn,rhn->bqhr', qa[..., ROPE_DIM:], w_uk)
        s = (jnp.einsum('bqhr,bqkr->bhqk', q_lat, c_sel)
             + jnp.einsum('bqhe,bqke->bhqk', qa[..., :ROPE_DIM], r_sel)) * HEAD_DIM ** -0.5
        s = jnp.where(valid[:, None], s.astype(jnp.float32), -jnp.inf)
        p = jax.nn.softmax(s, axis=-1).astype(c_sel.dtype)
        o_lat = jnp.einsum('bhqk,bqkr->bqhr', p, c_sel)
        return jnp.einsum('bqhr,rhd->bqhd', o_lat, w_uv)

    out = lax.map(block, jnp.arange(S // Q_BLOCK))
    return out.transpose(1, 0, 2, 3, 4).reshape(B, S, WIDTH_A)


def moba_attention(q, k, v):
    B, S, H, Dh = q.shape
    n_kb = -(-S // MOBA_BLOCK)
    pad = n_kb * MOBA_BLOCK - S

    def to_blocks(t):
        t = jnp.pad(t, ((0, 0), (0, pad), (0, 0), (0, 0)))
        return t.reshape(B, n_kb, MOBA_BLOCK, H, Dh).transpose(0, 3, 1, 2, 4)

    k_blk, v_blk = to_blocks(k), to_blocks(v)
    k_mean = jnp.mean(k_blk.astype(jnp.float32), axis=3).astype(q.dtype)
    n_sel = min(MOBA_TOPK, n_kb - 1)
    q_t = q.transpose(0, 2, 1, 3)
    b_ix = jnp.arange(B)[:, None, None, None]
    h_ix = jnp.arange(H)[None, :, None, None]
    blk_ids = jnp.arange(n_kb)
    scale = Dh ** -0.5

    def block(i):
        start = i * Q_BLOCK
        q_pos = start + jnp.arange(Q_BLOCK)
        own = start // MOBA_BLOCK
        qc = lax.dynamic_slice_in_dim(q_t, start, Q_BLOCK, axis=2)
        k_own = lax.dynamic_index_in_dim(k_blk, own, axis=2, keepdims=False)
        v_own = lax.dynamic_index_in_dim(v_blk, own, axis=2, keepdims=False)
        own_pos = own * MOBA_BLOCK + jnp.arange(MOBA_BLOCK)
        s_own = jnp.einsum('bhqd,bhkd->bhqk', qc, k_own).astype(jnp.float32) * scale
        s_own = jnp.where(own_pos[None, :] <= q_pos[:, None], s_own, -jnp.inf)
        if n_sel == 0:
            p = jax.nn.softmax(s_own, axis=-1).astype(v_own.dtype)
            return jnp.einsum('bhqk,bhkd->bhqd', p, v_own)
        gate = jnp.einsum('bhqd,bhnd->bhqn', qc, k_mean).astype(jnp.float32)
        gate = jnp.where(blk_ids < own, gate, -jnp.inf)
        _, sel = lax.top_k(gate, n_sel)
        sel_valid = sel < own
        k_sel = k_blk[b_ix, h_ix, sel]
        v_sel = v_blk[b_ix, h_ix, sel]
        s_sel = jnp.einsum('bhqd,bhqnkd->bhqnk', qc, k_sel).astype(jnp.float32) * scale
        s_sel = jnp.where(sel_valid[..., None], s_sel, -jnp.inf)
        s = jnp.concatenate([s_sel.reshape(B, H, Q_BLOCK, n_sel * MOBA_BLOCK), s_own], axis=-1)
        p = jax.nn.softmax(s, axis=-1).astype(v_own.dtype)
        p_sel = p[..., :n_sel * MOBA_BLOCK].reshape(B, H, Q_BLOCK, n_sel, MOBA_BLOCK)
        p_own = p[..., n_sel * MOBA_BLOCK:]
        return (jnp.einsum('bhqnk,bhqnkd->bhqd', p_sel, v_sel)
                + jnp.einsum('bhqk,bhkd->bhqd', p_own, v_own))

    out = lax.map(block, jnp.arange(S // Q_BLOCK))
    return out.transpose(1, 0, 3, 2, 4).reshape(B, S, H * Dh)


def hybrid_mixer(h, pos, w_in, kv_norm_g, w_uk, w_uv, w_branch_a, w_branch_b, w_out):
    B, S, _ = h.shape
    proj = h @ w_in
    (q_a, ckv, k_rope, q_idx, k_idx, w_idx, q_b, k_b, v_b, gate_a, gate_b) = jnp.split(
        proj, np.cumsum(IN_SPLITS)[:-1].tolist(), axis=-1)
    q_a = partial_rotary(q_a.reshape(B, S, N_HEADS_A, HEAD_DIM), pos)
    ckv = rms_norm(ckv, kv_norm_g)
    k_rope = partial_rotary(k_rope, pos)
    q_idx = partial_rotary(q_idx.reshape(B, S, IDX_HEADS, IDX_DIM), pos)
    k_idx = partial_rotary(k_idx, pos)
    o_a = dsa_attention(q_a, ckv, k_rope, q_idx, k_idx, w_idx, w_uk, w_uv)
    q_b = partial_rotary(q_b.reshape(B, S, N_HEADS_B, HEAD_DIM), pos)
    k_b = partial_rotary(k_b.reshape(B, S, N_HEADS_B, HEAD_DIM), pos)
    v_b = v_b.reshape(B, S, N_HEADS_B, HEAD_DIM)
    o_b = moba_attention(q_b, k_b, v_b)
    y = jax.nn.sigmoid(gate_a) * (o_a @ w_branch_a) + jax.nn.sigmoid(gate_b) * (o_b @ w_branch_b)
    return y @ w_out


def setup_inputs(seed: int = 0) -> dict:
    key = jax.random.key(seed)
    ks = jax.random.split(key, 20)

    def nrm(k, shape, scale):
        return jax.random.normal(k, shape, jnp.float32) * scale

    return {
        'x': nrm(ks[0], (BATCH, SEQ, D_MODEL), 1.0),
        'c': nrm(ks[1], (BATCH, D_MODEL), 1.0),
        'positions': jnp.tile(jnp.arange(SEQ, dtype=jnp.int32)[None, :], (BATCH, 1)),
        'ada_w': nrm(ks[2], (DEPTH, D_MODEL, N_MOD * D_MODEL), D_MODEL ** -0.5),
        'ada_b': nrm(ks[3], (DEPTH, N_MOD * D_MODEL), 0.02),
        'norm1_g': 1.0 + nrm(ks[4], (DEPTH, D_MODEL), 0.05),
        'ffn1_w_in': nrm(ks[5], (DEPTH, D_MODEL, 2 * D_FF), D_MODEL ** -0.5),
        'ffn1_w_out': nrm(ks[6], (DEPTH, D_FF, D_MODEL), D_FF ** -0.5),
        'norm2_g': 1.0 + nrm(ks[7], (DEPTH, D_MODEL), 0.05),
        'w_in': nrm(ks[8], (DEPTH, D_MODEL, IN_COLS), D_MODEL ** -0.5),
        'kv_norm_g': 1.0 + nrm(ks[9], (DEPTH, KV_RANK), 0.05),
        'w_uk': nrm(ks[10], (DEPTH, KV_RANK, N_HEADS_A, NOPE_DIM), KV_RANK ** -0.5),
        'w_uv': nrm(ks[11], (DEPTH, KV_RANK, N_HEADS_A, HEAD_DIM), KV_RANK ** -0.5),
        'w_branch_a': nrm(ks[12], (DEPTH, WIDTH_A, D_MODEL), WIDTH_A ** -0.5),
        'w_branch_b': nrm(ks[13], (DEPTH, WIDTH_B, D_MODEL), WIDTH_B ** -0.5),
        'w_out': nrm(ks[14], (DEPTH, D_MODEL, D_MODEL), D_MODEL ** -0.5),
        'norm3_g': 1.0 + nrm(ks[15], (DEPTH, D_MODEL), 0.05),
        'ffn2_w_in': nrm(ks[16], (DEPTH, D_MODEL, 2 * D_FF), D_MODEL ** -0.5),
        'ffn2_w_out': nrm(ks[17], (DEPTH, D_FF, D_MODEL), D_FF ** -0.5),
        'final_g': 1.0 + nrm(ks[18], (D_MODEL,), 0.05),
    }


def reference(x, c, positions, ada_w, ada_b, norm1_g, ffn1_w_in, ffn1_w_out, norm2_g, w_in,
              kv_norm_g, w_uk, w_uv, w_branch_a, w_branch_b, w_out, norm3_g, ffn2_w_in,
              ffn2_w_out, final_g):
    B = x.shape[0]
    c_act = jax.nn.silu(c)
    for l in range(DEPTH):
        mod = (c_act @ ada_w[l] + ada_b[l]).reshape(B, N_MOD, 1, D_MODEL)
        sh1, sc1, g1 = mod[:, 0], mod[:, 1], mod[:, 2]
        sh2, sc2, g2 = mod[:, 3], mod[:, 4], mod[:, 5]
        sh3, sc3, g3 = mod[:, 6], mod[:, 7], mod[:, 8]
        h = modulate(rms_norm(x, norm1_g[l]), sh1, sc1)
        x = x + 0.5 * g1 * swiglu(h, ffn1_w_in[l], ffn1_w_out[l])
        h = modulate(rms_norm(x, norm2_g[l]), sh2, sc2)
        x = x + g2 * hybrid_mixer(h, positions, w_in[l], kv_norm_g[l], w_uk[l], w_uv[l],
                                  w_branch_a[l], w_branch_b[l], w_out[l])
        h = modulate(rms_norm(x, norm3_g[l]), sh3, sc3)
        x = x + 0.5 * g3 * swiglu(h, ffn2_w_in[l], ffn2_w_out[l])
    return rms_norm(x, final_g)
```

```python
import math
from contextlib import ExitStack, contextmanager

import numpy as np
import concourse.bass as bass
import concourse.mybir as mybir
from concourse.bass_utils import run_bass_kernel_spmd

F32 = mybir.dt.float32
BF16 = mybir.dt.bfloat16
I32 = mybir.dt.int32
ALU = mybir.AluOpType
AF = mybir.ActivationFunctionType
AX = mybir.AxisListType

D = 1024
SEQ = 8192
NTT = 16
NOT = 4
DFF = 2816
NF = 22
EPS = 1e-6
NIT = 18
BIG = 1.0e30
TWO_PI = 2.0 * math.pi
KC = 1232
KRC = 160
QC = 1544
QRC = 384


class Sched:
    ENGS = ('pe', 'act', 'dve', 'pool', 'sp')

    def __init__(self, nc):
        self.nc = nc
        self.eng = {'pe': nc.tensor, 'act': nc.scalar, 'dve': nc.vector,
                    'pool': nc.gpsimd, 'sp': nc.sync}
        self.n_dma = {'sp': 12, 'pool': 8}
        self.stack = ExitStack()
        self.pstack = [self.stack]
        self.sems = {}
        self.cnt = {}
        self.known = {e: {} for e in self.ENGS}
        self.res_w = {}
        self.res_r = {}
        self.dma_rr = {q: 0 for q in self.n_dma}
        self.uid = 0

    def ctx(self):
        st = self.stack
        st.__enter__()
        nc = self.nc
        for e in self.ENGS:
            self.sems[e] = st.enter_context(nc.semaphore("s_" + e))
            self.cnt[e] = 0
        for q, n in self.n_dma.items():
            for i in range(n):
                k = "d_%s%d" % (q, i)
                self.sems[k] = st.enter_context(nc.semaphore(k))
                self.cnt[k] = 0
        self.psum = st.enter_context(nc.psum_tensor("psum_all", [128, 8, 512], F32))
        return st

    def sb(self, name, shape, dtype):
        self.uid += 1
        return self.pstack[-1].enter_context(
            self.nc.sbuf_tensor("%s_%d" % (name, self.uid), list(shape), dtype))

    @contextmanager
    def phase(self):
        self.barrier()
        for e in self.ENGS:
            self.uid += 1
            self.sems[e] = self.stack.enter_context(self.nc.semaphore("s_%s_%d" % (e, self.uid)))
            self.cnt[e] = 0
            for x in self.ENGS:
                self.known[x][e] = 0
        st = ExitStack()
        st.__enter__()
        self.pstack.append(st)
        try:
            yield
        finally:
            self.barrier()
            self.pstack.pop()
            st.__exit__(None, None, None)

    def _deps(self, reads, writes):
        deps = {}
        for r in reads:
            if r in self.res_w:
                s, v = self.res_w[r]
                if deps.get(s, 0) < v:
                    deps[s] = v
        for w in writes:
            if w in self.res_w:
                s, v = self.res_w[w]
                if deps.get(s, 0) < v:
                    deps[s] = v
            for s, v in self.res_r.get(w, {}).items():
                if deps.get(s, 0) < v:
                    deps[s] = v
        return deps

    def _emit_waits(self, e, deps):
        eng = self.eng[e]
        for s, v in deps.items():
            if e == 'pe' and s == 'pe':
                continue
            if self.known[e].get(s, 0) < v:
                eng.wait_ge(self.sems[s], v)
                self.known[e][s] = v

    def _record(self, ev, reads, writes):
        s, v = ev
        for r in reads:
            d = self.res_r.setdefault(r, {})
            if d.get(s, 0) < v:
                d[s] = v
        for w in writes:
            self.res_w[w] = ev
            self.res_r[w] = {}

    def op(self, e, fn, reads=(), writes=()):
        self._emit_waits(e, self._deps(reads, writes))
        ins = fn(self.eng[e])
        self.cnt[e] += 1
        ins.then_inc(self.sems[e], 1)
        self._record((e, self.cnt[e]), reads, writes)

    def dma(self, q, out, in_, reads=(), writes=()):
        deps = self._deps(reads, writes)
        n = self.n_dma[q]
        i = self.dma_rr[q]
        self.dma_rr[q] = (i + 1) % n
        k = "d_%s%d" % (q, i)
        if deps.get(k, 0) < self.cnt[k]:
            deps[k] = self.cnt[k]
        self._emit_waits(q, deps)
        ins = self.eng[q].dma_start(out=out, in_=in_)
        self.cnt[k] += 16
        ins.then_inc(self.sems[k], 16)
        self._record((k, self.cnt[k]), reads, writes)

    def barrier(self):
        for e in self.ENGS:
            deps = {s: v for s, v in self.cnt.items() if v > 0 and s != e}
            self._emit_waits(e, deps)
        self.res_w = {}
        self.res_r = {}


def build_nc(dbg=False, stop_after=99, start_at=0, ntt_b=NTT, not_b=NOT, skip=()):
    nc = bass.Bass("TRN2", target_bir_lowering=False)

    def din(name, shape, dt=F32):
        return nc.dram_tensor(name, list(shape), dt, kind="ExternalInput").ap()

    def dscr(name, shape, dt):
        kind = "ExternalOutput" if dbg else "Internal"
        return nc.dram_tensor(name, list(shape), dt, kind=kind).ap()

    xT = din("xT", [D, SEQ])
    xoT = din("xoT", [D, 2048])
    cT = din("cT", [128, 8])
    posb = din("posb", [1, SEQ], I32)
    poso = din("poso", [1, 2048], I32)
    invf = din("invf", [16, 1])
    identd = din("identd", [128, 128])
    pwd = din("pwd", [128, NIT])
    cnegd = din("cnegd", [128, 512])
    cmTd = din("cmTd", [128, 512])
    flagd = din("flagd", [128, 2])
    ada_w = din("ada_w", [D, 9 * D])
    ada_bT = din("ada_bT", [128, 72])
    gnd = din("gnd", [128, 32])
    gkvd = din("gkvd", [128, 1])
    w1i = din("w1i", [D, 2 * DFF])
    w1o = din("w1o", [DFF, D])
    w2i = din("w2i", [D, 2 * DFF])
    w2o = din("w2o", [DFF, D])
    wkd = din("wkd", [D, KC])
    wkrd = din("wkrd", [D, KRC])
    wqd = din("wqd", [D, QC])
    wqrd = din("wqrd", [D, QRC])
    wgd = din("wgd", [D, 2048])
    wukTd = din("wukTd", [48, 8 * 128])
    wuvd = din("wuvd", [128, 512])
    wbad = din("wbad", [512, D])
    wbbd = din("wbbd", [512, D])
    wod = din("wod", [D, D])
    outT = nc.dram_tensor("outT", [D, 2048], F32, kind="ExternalOutput").ap()

    h2T_d = dscr("h2T_d", [D, SEQ], BF16)
    h2oT_d = dscr("h2oT_d", [D, 2048], BF16)
    x1oT_d = dscr("x1oT_d", [D, 2048], F32)
    x2oT_d = dscr("x2oT_d", [D, 2048], F32)
    ckvT_d = dscr("ckvT_d", [128, SEQ], BF16)
    ckvtok_d = dscr("ckvtok_d", [64, 128, 128], BF16)
    kropeT_d = dscr("kropeT_d", [16, SEQ], BF16)
    kidxT_d = dscr("kidxT_d", [64, SEQ], BF16)
    kbT_d = dscr("kbT_d", [8, 64, SEQ], BF16)
    vaug_d = dscr("vaug_d", [2, 64, 128, 320], BF16)
    qlat_d = dscr("qlat_d", [16, 128, 1024], BF16)
    qrope_d = dscr("qrope_d", [16, 16, 1024], BF16)
    qidx_d = dscr("qidx_d", [16, 64, 1024], BF16)
    qbb_d = dscr("qbb_d", [16, 64, 1024], BF16)
    qbf_d = dscr("qbf_d", [16, 64, 1024], F32)
    kmean_dbg = dscr("kmean_dbg", [64, 256], F32)
    widx_dbg = dscr("widx_dbg", [128, 128], F32)
    oaT_d = dscr("oaT_d", [128, 4 * 2048], BF16)
    obT_d = dscr("obT_d", [128, 4 * 2048], BF16)

    import os
    DIS = set(os.environ.get('KDIS', '').split(','))
    if 'Q' in DIS:
        not_b = 0
    S = Sched(nc)
    with S.ctx():
        ps = S.psum
        op, dma = S.op, S.dma
        bankc = [0]

        def nbank():
            b = bankc[0]
            bankc[0] = (b + 1) % 8
            return b

        def P(b):
            return 'ps%d' % b

        identb = S.sb("identb", [128, 128], BF16)
        onesb = S.sb("onesb", [128, 128], BF16)
        modT = S.sb("modT", [128, 72], F32)
        cst = S.sb("cst", [128, 9, 8], F32)
        gfin = S.sb("gfin", [128, 8], F32)
        gkvs = S.sb("gkvs", [128, 1], F32)
        invf_s = S.sb("invf_s", [16, 1], F32)
        kmean = S.sb("kmean", [64, 8, 32], F32)
        widx_all = S.sb("widx_all", [128, 16, 8], F32)
        flag_s = S.sb("flag_s", [128, 2], F32)

        dma('pool', identb[:], identd, writes=['identb'])
        op('dve', lambda e: e.memset(widx_all[:], 0.0), writes=['widx_all'])
        op('dve', lambda e: e.memset(onesb[:], 1.0), writes=['onesb'])
        dma('sp', invf_s[:], invf, writes=['invf_s'])
        dma('sp', flag_s[:], flagd, writes=['flag_s'])

        def rsqrt_from(ps_ap, out_ap, addc, rd, wr):
            op('act', lambda e: e.activation(out_ap, ps_ap, AF.Ln, bias=addc_ap(addc), scale=1.0), reads=rd + ['cbias'], writes=wr)
            op('act', lambda e: e.activation(out_ap, out_ap, AF.Exp, scale=-0.5), reads=wr, writes=wr)

        cbias = S.sb("cbias", [128, 4], F32)
        op('dve', lambda e: e.memset(cbias[:, 0:1], 1024.0 * EPS), writes=['cbias'])
        op('dve', lambda e: e.memset(cbias[:, 1:2], 128.0 * EPS), writes=['cbias'])
        op('dve', lambda e: e.memset(cbias[:, 2:3], 0.0), writes=['cbias'])

        def addc_ap(which):
            return cbias[:, which:which + 1]

        with (S.phase() if start_at <= 0 else ExitStack()):
          if start_at <= 0:
              c_s = S.sb("c_s", [128, 8], F32)
              cact = S.sb("cact", [128, 8], BF16)
              abT = S.sb("abT", [128, 72], F32)
              gn_s = S.sb("gn_s", [128, 4, 8], F32)
              gkv_s = S.sb("gkv_s", [128, 1], F32)
              awb = [S.sb("awb%d" % i, [128, 8, 1024], BF16) for i in range(2)]
              dma('sp', c_s[:], cT, writes=['c_s'])
              dma('sp', abT[:], ada_bT, writes=['abT'])
              dma('sp', gn_s[:].rearrange("p a b -> p (a b)"), gnd, writes=['gn_s'])
              dma('sp', gkv_s[:], gkvd, writes=['gkv_s'])
              op('act', lambda e: e.activation(cact[:], c_s[:], AF.Silu), reads=['c_s'], writes=['cact'])
              awv = ada_w.rearrange("(j p) c -> p j c", p=128)
              for m in range(9):
                  a = awb[m % 2]
                  an = 'awb%d' % (m % 2)
                  for j in range(8):
                      dma('pool', a[:, j, :], awv[:, j, m * 1024:(m + 1) * 1024], writes=[an])
                  for kc in range(8):
                      col = m * 8 + kc
                      for j in range(8):
                          op('pe', lambda e, a=a, kc=kc, j=j, col=col: e.matmul(
                              ps[:, 0, col:col + 1], a[:, j, kc * 128:(kc + 1) * 128], cact[:, j:j + 1],
                              start=(j == 0), stop=(j == 7)), reads=[an, 'cact'], writes=['ps0'])
              op('dve', lambda e: e.tensor_tensor(modT[:], ps[:, 0, 0:72], abT[:], ALU.add), reads=['ps0', 'abT'], writes=['modT'])
              mv = modT[:].rearrange("p (m k) -> p m k", k=8)
              for n_i, (msh, msc, mg, gscale) in enumerate([(0, 1, 2, 0.5), (3, 4, 5, 1.0), (6, 7, 8, 0.5)]):
                  op('dve', lambda e, n_i=n_i, msc=msc: e.scalar_tensor_tensor(
                      cst[:, 3 * n_i, :], mv[:, msc, :], 1.0, gn_s[:, n_i, :], ALU.add, ALU.mult),
                     reads=['modT', 'gn_s'], writes=['cst'])
                  op('dve', lambda e, n_i=n_i: e.tensor_scalar(cst[:, 3 * n_i, :], cst[:, 3 * n_i, :], 32.0, None, ALU.mult),
                     reads=['cst'], writes=['cst'])
                  op('dve', lambda e, n_i=n_i, msh=msh: e.tensor_copy(cst[:, 3 * n_i + 1, :], mv[:, msh, :]),
                     reads=['modT', 'cst'], writes=['cst'])
                  op('dve', lambda e, n_i=n_i, mg=mg, gscale=gscale: e.tensor_scalar(
                      cst[:, 3 * n_i + 2, :], mv[:, mg, :], gscale, None, ALU.mult), reads=['modT', 'cst'], writes=['cst'])
              op('dve', lambda e: e.tensor_scalar(gfin[:], gn_s[:, 3, :], 32.0, None, ALU.mult), reads=['gn_s'], writes=['gfin'])
              op('dve', lambda e: e.tensor_scalar(gkvs[:], gkv_s[:], math.sqrt(128.0), None, ALU.mult), reads=['gkv_s'], writes=['gkvs'])

        def norm_mod(xt, xn, gsc, sh, ht, hn, sq, rp, tmp):
            b = 6
            for k in range(8):
                op('act', lambda e, k=k: e.activation(sq[:, k % 2, :], xt[:, k, :], AF.Square), reads=[xn], writes=['sq%d' % (k % 2)])
                op('pe', lambda e, k=k: e.matmul(ps[:, b, :], onesb[:], sq[:, k % 2, :], start=(k == 0), stop=(k == 7)),
                   reads=['sq%d' % (k % 2), 'onesb'], writes=[P(b)])
            rsqrt_from(ps[:, b, :], rp[:], 0, [P(b)], ['rp'])
            for k in range(8):
                op('dve', lambda e, k=k: e.tensor_tensor(tmp[:, k % 2, :], xt[:, k, :], rp[:], ALU.mult),
                   reads=[xn, 'rp'], writes=['tmp%d' % (k % 2)])
                if sh is None:
                    op('act', lambda e, k=k: e.activation(ht[:, k, :], tmp[:, k % 2, :], AF.Identity,
                                                          bias=cbias[:, 2:3], scale=gsc[:, k:k + 1]),
                       reads=['tmp%d' % (k % 2), 'cst', 'gfin', 'cbias'], writes=[hn])
                else:
                    op('act', lambda e, k=k: e.activation(ht[:, k, :], tmp[:, k % 2, :], AF.Identity,
                                                          bias=sh[:, k:k + 1], scale=gsc[:, k:k + 1]),
                       reads=['tmp%d' % (k % 2), 'cst'], writes=[hn])

        def ffn_tile(xt, xn, gsc, sh, hg, wi, wo, xo, xon, W):
            hT, gT, sa, sq, rp, tmp = W['hT'], W['gT'], W['sa'], W['sq'], W['rp'], W['tmp']
            norm_mod(xt, xn, gsc, sh, hT, 'hT', sq, rp, tmp)
            for f in range(NF):
                bA, bB = f % 2, 2 + f % 2
                for k in range(8):
                    op('pe', lambda e, k=k, f=f: e.matmul(ps[:, bA, :], wi[:, k, f * 128:(f + 1) * 128], hT[:, k, :],
                                                          start=(k == 0), stop=(k == 7)), reads=['wi', 'hT'], writes=[P(bA)])
                for k in range(8):
                    op('pe', lambda e, k=k, f=f: e.matmul(ps[:, bB, :], wi[:, k, DFF + f * 128:DFF + (f + 1) * 128], hT[:, k, :],
                                                          start=(k == 0), stop=(k == 7)), reads=['wi', 'hT'], writes=[P(bB)])
                s_ = sa[f % 2]
                op('act', lambda e, s_=s_: e.activation(s_[:], ps[:, bA, :], AF.Silu), reads=[P(bA)], writes=['sa%d' % (f % 2)])
                op('dve', lambda e, s_=s_, f=f: e.tensor_tensor(gT[:, f, :], s_[:], ps[:, bB, :], ALU.mult),
                   reads=['sa%d' % (f % 2), P(bB)], writes=['gT%d' % f])
            for d in range(8):
                bO = 4 + d % 2
                for f in range(NF):
                    op('pe', lambda e, d=d, f=f: e.matmul(ps[:, bO, :], wo[:, f, d * 128:(d + 1) * 128], gT[:, f, :],
                                                          start=(f == 0), stop=(f == NF - 1)), reads=['wo', 'gT%d' % f], writes=[P(bO)])
                op('dve', lambda e, d=d: e.scalar_tensor_tensor(xo[:, d, :], ps[:, bO, :], hg[:, d:d + 1], xt[:, d, :],
                                                                ALU.mult, ALU.add), reads=[P(bO), xn, 'cst'], writes=[xon])

        def load_ffn_w(wi_d, wo_d, wi, wo):
            wiv = wi_d.rearrange("(k p) c -> p k c", p=128)
            for k in range(8):
                for hf in range(2):
                    dma('pool', wi[:, k, hf * DFF:(hf + 1) * DFF], wiv[:, k, hf * DFF:(hf + 1) * DFF], writes=['wi'])
            wov = wo_d.rearrange("(f p) c -> p f c", p=128)
            for f in range(NF):
                dma('pool', wo[:, f, :], wov[:, f, :], writes=['wo'])

        def ffn_work():
            return dict(hT=S.sb("hT", [128, 8, 512], BF16), gT=S.sb("gT", [128, NF, 512], BF16),
                        sa=[S.sb("sa%d" % i, [128, 512], BF16) for i in range(2)],
                        sq=S.sb("sq", [128, 2, 512], BF16), rp=S.sb("rp", [128, 512], F32),
                        tmp=S.sb("tmp", [128, 2, 512], F32))

        if stop_after >= 1 and start_at <= 1 and 1 not in skip:
            with S.phase():
                wi = S.sb("wi", [128, 8, 2 * DFF], BF16)
                wo = S.sb("wo", [128, NF, D], BF16)
                load_ffn_w(w1i, w1o, wi, wo)
                W = ffn_work()
                xb = [S.sb("xb%d" % i, [128, 8, 512], F32) for i in range(2)]
                xTv = xT.rearrange("(k p) t -> p k t", p=128)
                xoTv = xoT.rearrange("(k p) t -> p k t", p=128)
                h2v = h2T_d.rearrange("(k p) t -> p k t", p=128)
                h2ov = h2oT_d.rearrange("(k p) t -> p k t", p=128)
                x1ov = x1oT_d.rearrange("(k p) t -> p k t", p=128)
                ntiles = NTT + NOT
                for tt in range(ntiles):
                    own = tt >= NTT
                    src = xoTv[:, :, (tt - NTT) * 512:(tt - NTT + 1) * 512] if own else xTv[:, :, tt * 512:(tt + 1) * 512]
                    xt = xb[tt % 2]
                    xn = 'xb%d' % (tt % 2)
                    dma('sp', xt[:], src, writes=[xn])
                    ffn_tile(xt, xn, cst[:, 0, :], cst[:, 1, :], cst[:, 2, :], wi, wo, xt, xn, W)
                    x1, h2 = xt, W['hT']
                    norm_mod(xt, xn, cst[:, 3, :], cst[:, 4, :], h2, 'hT', W['sq'], W['rp'], W['tmp'])
                    if own:
                        c0 = (tt - NTT) * 512
                        dma('sp', h2ov[:, :, c0:c0 + 512], h2[:], reads=['hT'], writes=['h2o_d'])
                        dma('sp', x1ov[:, :, c0:c0 + 512], x1[:], reads=[xn], writes=['x1o_d'])
                    else:
                        dma('sp', h2v[:, :, tt * 512:(tt + 1) * 512], h2[:], reads=['hT'], writes=['h2_d'])

        def rope_tables(pos_ap, pi, pf, Ct, St, qi, qf, t2):
            dma('sp', pi[:], pos_ap.partition_broadcast(16), writes=['pi'])
            op('dve', lambda e: e.tensor_copy(pf[:], pi[:]), reads=['pi'], writes=['pf'])
            op('dve', lambda e: e.tensor_scalar(pf[:], pf[:], invf_s[:, 0:1], None, ALU.mult), reads=['pf', 'invf_s'], writes=['pf'])
            for tab, tn, off in ((St, 'St', 0.0), (Ct, 'Ct', math.pi / 2)):
                op('dve', lambda e, off=off: e.tensor_scalar(tab[:], pf[:], off, None, ALU.add), reads=['pf'], writes=[tn])
                op('dve', lambda e: e.tensor_scalar(qf[:], tab[:], 1.0 / TWO_PI, None, ALU.mult), reads=[tn], writes=['qf'])
                op('dve', lambda e: e.tensor_copy(qi[:], qf[:]), reads=['qf'], writes=['qi'])
                op('dve', lambda e: e.tensor_copy(qf[:], qi[:]), reads=['qi'], writes=['qf'])
                op('dve', lambda e: e.scalar_tensor_tensor(tab[:], qf[:], -TWO_PI, tab[:], ALU.mult, ALU.add), reads=['qf', tn], writes=[tn])
                op('dve', lambda e: e.tensor_scalar(t2[:], tab[:], math.pi, -TWO_PI, ALU.is_gt, ALU.mult), reads=[tn], writes=['t2'])
                op('dve', lambda e: e.tensor_tensor(tab[:], tab[:], t2[:], ALU.add), reads=[tn, 't2'], writes=[tn])
                op('dve', lambda e: e.tensor_scalar(t2[:], tab[:], -math.pi, TWO_PI, ALU.is_lt, ALU.mult), reads=[tn], writes=['t2'])
                op('dve', lambda e: e.tensor_tensor(tab[:], tab[:], t2[:], ALU.add), reads=[tn, 't2'], writes=[tn])
                op('act', lambda e: e.activation(tab[:], tab[:], AF.Sin), reads=[tn], writes=[tn])

        if stop_after >= 2:
            with S.phase():
                wk = S.sb("wk", [128, 8, KC], BF16)
                wkh = S.sb("wkh", [128, 8, KRC], BF16)
                wkr = S.sb("wkr", [128, 8, KRC], BF16)
                wq = S.sb("wq", [128, 8, QC + 64], BF16)
                op('dve', lambda e: e.memset(wq[:], 0.0), writes=['wq'])
                wqh = S.sb("wqh", [128, 8, QRC], BF16)
                wqr = S.sb("wqr", [128, 8, QRC], BF16)
                wuk = S.sb("wuk", [64, 8, 128], BF16)
                op('dve', lambda e: e.memset(wuk[:], 0.0), writes=['wuk'])
                for (dst, src_d, nm) in ((wk, wkd, 'wk'), (wkr, wkrd, 'wkr'), (wq, wqd, 'wq'), (wqr, wqrd, 'wqr')):
                    sv = src_d.rearrange("(k p) c -> p k c", p=128)
                    for k in range(8):
                        dma('pool', dst[:, k, 0:sv.shape[2]], sv[:, k, :], reads=[nm], writes=[nm])
                dma('pool', wuk[0:48, :, :].rearrange("p a b -> p (a b)"), wukTd, reads=['wuk'], writes=['wuk'])
                for (wr_, wh_, nr, nh, ng) in ((wkr, wkh, 'wkr', 'wkh', KRC // 16), (wqr, wqh, 'wqr', 'wqh', QRC // 16)):
                    for k in range(8):
                        rv = wr_[:, k, :].rearrange("p (g two e) -> p g two e", two=2, e=8)
                        hv = wh_[:, k, :].rearrange("p (g two e) -> p g two e", two=2, e=8)
                        op('dve', lambda e, rv=rv, hv=hv: e.tensor_scalar(hv[:, :, 0, :], rv[:, :, 1, :], -1.0, None, ALU.mult),
                           reads=[nr], writes=[nh])
                        op('dve', lambda e, rv=rv, hv=hv: e.tensor_copy(hv[:, :, 1, :], rv[:, :, 0, :]), reads=[nr], writes=[nh])

                h2 = S.sb("h2", [128, 8, 512], BF16)
                pi = S.sb("pi", [16, 512], I32)
                pf = S.sb("pf", [16, 512], F32)
                qi = S.sb("qi", [16, 512], I32)
                qf = S.sb("qf", [16, 512], F32)
                t2 = S.sb("t2", [16, 512], F32)
                Ct = S.sb("Ct", [16, 512], F32)
                St = S.sb("St", [16, 512], F32)
                r1 = S.sb("r1", [16, 512], F32)
                r2 = S.sb("r2", [16, 512], F32)
                sqk = S.sb("sqk", [128, 512], BF16)
                rk = S.sb("rk", [128, 512], F32)
                ckv_t = S.sb("ckv_t", [128, 512], BF16)
                ckvtok_t = S.sb("ckvtok_t", [128, 4, 128], BF16)
                krope_t = S.sb("krope_t", [16, 512], BF16)
                kf = S.sb("kf", [64, 512], F32)
                kb_t = [S.sb("kb_t%d" % i, [64, 512], BF16) for i in range(2)]
                vaug_t = S.sb("vaug_t", [128, 4, 8, 80], BF16)
                kmsum = S.sb("kmsum", [64, 8, 32], F32)
                kmjunk = S.sb("kmjunk", [64, 2], F32)
                op('dve', lambda e: e.memset(vaug_t[:], 1.0), writes=['vaug_t'])
                op('dve', lambda e: e.memset(kmsum[:], 0.0), writes=['kmsum'])

                def proj(lhs_w, c0, m, h2t, n=512):
                    b = nbank()
                    for k in range(8):
                        op('pe', lambda e, k=k: e.matmul(ps[0:m, b, 0:n], lhs_w[:, k, c0:c0 + m], h2t[:, k, 0:n],
                                                         start=(k == 0), stop=(k == 7)),
                           reads=['h2', 'wk', 'wkh', 'wq', 'wqh'], writes=[P(b)])
                    return b

                def rope_apply(bm, bh, out_ap, outn):
                    op('dve', lambda e: e.tensor_tensor(r1[:], ps[0:16, bm, :], Ct[:], ALU.mult), reads=[P(bm), 'Ct'], writes=['r1'])
                    op('dve', lambda e: e.tensor_tensor(r2[:], ps[0:16, bh, :], St[:], ALU.mult), reads=[P(bh), 'St'], writes=['r2'])
                    op('dve', lambda e: e.tensor_tensor(out_ap, r1[:], r2[:], ALU.add), reads=['r1', 'r2', outn], writes=[outn])

                h2v = h2T_d.rearrange("(k p) t -> p k t", p=128)
                for tt in range(ntt_b):
                    cols = slice(tt * 512, (tt + 1) * 512)
                    dma('sp', h2[:], h2v[:, :, cols], reads=['h2_d'], writes=['h2'])
                    rope_tables(posb[0:1, cols], pi, pf, Ct, St, qi, qf, t2)
                    if 'all' in DIS:
                        continue
                    if 'ckv' not in DIS:
                      b = proj(wk, 0, 128, h2)
                      op('act', lambda e, b=b: e.activation(sqk[:], ps[:, b, :], AF.Square), reads=[P(b)], writes=['sqk'])
                      b2 = nbank()
                      op('pe', lambda e, b2=b2: e.matmul(ps[:, b2, :], onesb[:], sqk[:], start=True, stop=True),
                         reads=['sqk', 'onesb'], writes=[P(b2)])
                      rsqrt_from(ps[:, b2, :], rk[:], 1, [P(b2)], ['rk'])
                      op('dve', lambda e, b=b: e.scalar_tensor_tensor(ckv_t[:], ps[:, b, :], gkvs[:, 0:1], rk[:], ALU.mult, ALU.mult),
                         reads=[P(b), 'rk', 'gkvs'], writes=['ckv_t'])
                      dma('sp', ckvT_d[:, cols], ckv_t[:], reads=['ckv_t'], writes=['ckvT_d'])
                      b3 = nbank()
                      for c in range(4):
                          op('pe', lambda e, c=c, b3=b3: e.matmul(ps[:, b3, c * 128:(c + 1) * 128], ckv_t[:, c * 128:(c + 1) * 128],
                                                                  identb[:], start=True, stop=True),
                             reads=['ckv_t', 'identb'], writes=[P(b3)])
                      op('act', lambda e, b3=b3: e.copy(ckvtok_t[:].rearrange("p a b -> p (a b)"), ps[:, b3, :]),
                         reads=[P(b3)], writes=['ckvtok_t'])
                      dma('sp', ckvtok_d[4 * tt:4 * tt + 4].rearrange("c p r -> p c r"), ckvtok_t[:], reads=['ckvtok_t'], writes=['ckvtok_d'])
                    if 'krope' not in DIS:
                      bm = proj(wk, 128, 16, h2)
                      bh = proj(wkh, 0, 16, h2)
                      rope_apply(bm, bh, krope_t[:], 'krope_t')
                      dma('sp', kropeT_d[:, cols], krope_t[:], reads=['krope_t'], writes=['kropeT_d'])
                    if 'kgrp' not in DIS:
                      for g in range(9):
                          c0 = 144 + 64 * g
                          bm = proj(wk, c0, 128, h2)
                          bh = proj(wkh, 16 + 16 * g, 16, h2)
                          op('dve', lambda e, bm=bm: e.tensor_copy(kf[:], ps[0:64, bm, :]), reads=[P(bm)], writes=['kf'])
                          rope_apply(bm, bh, kf[0:16, :], 'kf')
                          kt = kb_t[g % 2]
                          kn = 'kb_t%d' % (g % 2)
                          op('act', lambda e, kt=kt: e.copy(kt[:], kf[:]), reads=['kf'], writes=[kn])
                          if g > 0:
                              for hv_ in range(2):
                                  op('dve', lambda e, hv_=hv_, g=g, tt=tt: e.tensor_reduce(
                                      kmsum[:, g - 1, 2 * tt + hv_:2 * tt + hv_ + 1], kf[:, hv_ * 256:(hv_ + 1) * 256], AX.X, ALU.add),
                                     reads=['kf', 'kmsum'], writes=['kmsum'])
                          if g == 0:
                              dma('sp', kidxT_d[:, cols], kt[:], reads=[kn], writes=['kidxT_d'])
                          else:
                              h = g - 1
                              dma('sp', kbT_d[h, :, cols], kt[:], reads=[kn], writes=['kbT_d'])
                              pass
                    if 'v' not in DIS:
                      for c in range(4):
                          b = nbank()
                          for k in range(8):
                              op('pe', lambda e, k=k, c=c, b=b: e.matmul(ps[:, b, :], h2[:, k, c * 128:(c + 1) * 128], wk[:, k, 720:1232],
                                                                         start=(k == 0), stop=(k == 7)), reads=['h2', 'wk'], writes=[P(b)])
                          op('act', lambda e, c=c, b=b: e.copy(vaug_t[:, c, :, 0:64], ps[:, b, :].rearrange("p (h d) -> p h d", d=64)),
                             reads=[P(b)], writes=['vaug_t'])
                      for hf in range(2):
                          dma('sp', vaug_d[hf, 4 * tt:4 * tt + 4].rearrange("c p x -> p c x"),
                              vaug_t[:, :, 4 * hf:4 * hf + 4, :].rearrange("p c h e -> p c (h e)"), reads=['vaug_t'], writes=['vaug_d'])
                op('dve', lambda e: e.tensor_scalar(kmean[:], kmsum[:], 1.0 / 256.0, None, ALU.mult), reads=['kmsum'], writes=['kmean'])

                qn = S.sb("qn", [64, 512], BF16)
                op('dve', lambda e: e.memset(qn[:], 0.0), writes=['qn'])
                qlat_st = S.sb("qlat_st", [128, 4, 8, 128], BF16)
                qrope_st = S.sb("qrope_st", [16, 4, 8, 128], BF16)
                qidx_st = S.sb("qidx_st", [64, 4, 8, 128], BF16)
                qbb_st = S.sb("qbb_st", [64, 4, 8, 128], BF16)
                qbf_st = S.sb("qbf_st", [64, 4, 8, 128], F32)
                h2ov = h2oT_d.rearrange("(k p) t -> p k t", p=128)
                for ot in range(not_b):
                    cols = slice(ot * 512, (ot + 1) * 512)
                    dma('sp', h2[:], h2ov[:, :, cols], reads=['h2o_d'], writes=['h2'])
                    rope_tables(poso[0:1, cols], pi, pf, Ct, St, qi, qf, t2)
                    for h in range(8):
                        b = proj(wq, 48 * h, 128, h2)
                        op('act', lambda e, b=b: e.copy(qn[0:48, :], ps[0:48, b, :]), reads=[P(b), 'qn'], writes=['qn'])
                        b2 = nbank()
                        op('pe', lambda e, b2=b2, h=h: e.matmul(ps[:, b2, :], wuk[:, h, :], qn[:], start=True, stop=True),
                           reads=['wuk', 'qn'], writes=[P(b2)])
                        op('act', lambda e, b2=b2, h=h: e.copy(qlat_st[:, :, h, :], ps[:, b2, :].rearrange("p (a q) -> p a q", q=128)),
                           reads=[P(b2)], writes=['qlat_st'])
                        bm = proj(wq, 384 + 16 * h, 16, h2)
                        bh = proj(wqh, 16 * h, 16, h2)
                        rope_apply(bm, bh, kf[0:16, :], 'kf')
                        op('act', lambda e, h=h: e.copy(qrope_st[:, :, h, :], kf[0:16, :].rearrange("p (a q) -> p a q", q=128)),
                           reads=['kf'], writes=['qrope_st'])
                        bm = proj(wq, 512 + 64 * h, 128, h2)
                        bh = proj(wqh, 128 + 16 * h, 16, h2)
                        op('dve', lambda e, bm=bm: e.tensor_copy(kf[:], ps[0:64, bm, :]), reads=[P(bm)], writes=['kf'])
                        rope_apply(bm, bh, kf[0:16, :], 'kf')
                        op('act', lambda e, h=h: e.copy(qidx_st[:, :, h, :], kf[:].rearrange("p (a q) -> p a q", q=128)),
                           reads=['kf'], writes=['qidx_st'])
                        bm = proj(wq, 1032 + 64 * h, 128, h2)
                        bh = proj(wqh, 256 + 16 * h, 16, h2)
                        op('dve', lambda e, bm=bm: e.tensor_copy(kf[:], ps[0:64, bm, :]), reads=[P(bm)], writes=['kf'])
                        rope_apply(bm, bh, kf[0:16, :], 'kf')
                        op('act', lambda e, h=h: e.copy(qbb_st[:, :, h, :], kf[:].rearrange("p (a q) -> p a q", q=128)),
                           reads=['kf'], writes=['qbb_st'])
                        op('dve', lambda e, h=h: e.tensor_copy(qbf_st[:, :, h, :], kf[:].rearrange("p (a q) -> p a q", q=128)),
                           reads=['kf'], writes=['qbf_st'])
                    for c in range(4):
                        b = nbank()
                        for k in range(8):
                            op('pe', lambda e, k=k, c=c, b=b: e.matmul(ps[:, b, 0:8], h2[:, k, c * 128:(c + 1) * 128], wq[:, k, 1024:1032],
                                                                       start=(k == 0), stop=(k == 7)), reads=['h2', 'wq'], writes=[P(b)])
                        op('dve', lambda e, c=c, b=b, ot=ot: e.tensor_scalar(widx_all[:, 4 * ot + c, :], ps[:, b, 0:8],
                                                                             (8.0 ** -0.5) * (64.0 ** -0.5), None, ALU.mult),
                           reads=[P(b)], writes=['widx_all'])
                    for (st_, dd, nm) in ((qlat_st, qlat_d, 'qlat'), (qrope_st, qrope_d, 'qrope'), (qidx_st, qidx_d, 'qidx'),
                                          (qbb_st, qbb_d, 'qbb'), (qbf_st, qbf_d, 'qbf')):
                        dma('sp', dd[4 * ot:4 * ot + 4].rearrange("a p x -> p a x"), st_[:].rearrange("p a h q -> p a (h q)"),
                            reads=[nm + '_st'], writes=[nm + '_d'])

        if dbg and stop_after >= 2:
            dma('sp', kmean_dbg, kmean[:].rearrange("p a b -> p (a b)"), reads=['kmean'], writes=['kmean_dbg'])
            dma('sp', widx_dbg, widx_all[:].rearrange("p a b -> p (a b)"), reads=['widx_all'], writes=['widx_dbg'])
            S.barrier()
        if stop_after >= 3:
            with S.phase():
                kidx_s = S.sb("kidx_s", [64, SEQ], BF16)
                ckvT_s = S.sb("ckvT_s", [128, SEQ], BF16)
                krope_s = S.sb("krope_s", [16, SEQ], BF16)
                ckvtok_s = S.sb("ckvtok_s", [128, 64, 128], BF16)
                cneg = S.sb("cneg", [128, 512], F32)
                pw = S.sb("pw", [128, NIT], F32)
                wuv = S.sb("wuv", [128, 8, 64], BF16)
                wuvpad = S.sb("wuvpad", [128, 8, 128], BF16)
                for q4 in range(4):
                    cs = slice(q4 * 2048, (q4 + 1) * 2048)
                    dma('sp', kidx_s[:, cs], kidxT_d[:, cs], reads=['kidxT_d'], writes=['kidx_s'])
                    dma('sp', ckvT_s[:, cs], ckvT_d[:, cs], reads=['ckvT_d'], writes=['ckvT_s'])
                    dma('sp', krope_s[:, cs], kropeT_d[:, cs], reads=['kropeT_d'], writes=['krope_s'])
                    dma('sp', ckvtok_s[:, 16 * q4:16 * q4 + 16, :], ckvtok_d[16 * q4:16 * q4 + 16].rearrange("c p r -> p c r"),
                        reads=['ckvtok_d'], writes=['ckvtok_s'])
                dma('sp', cneg[:], cnegd, writes=['cneg'])
                dma('sp', pw[:], pwd, writes=['pw'])
                dma('pool', wuv[:].rearrange("p a b -> p (a b)"), wuvd, writes=['wuv'])
                op('dve', lambda e: e.memset(wuvpad[:], 0.0), writes=['wuvpad'])
                for h in range(8):
                    op('dve', lambda e, h=h: e.tensor_copy(wuvpad[:, h, (h % 2) * 64:(h % 2) * 64 + 64], wuv[:, h, :]),
                       reads=['wuv', 'wuvpad'], writes=['wuvpad'])

                qidx = S.sb("qidx", [64, 8, 128], BF16)
                qlat = S.sb("qlat", [128, 8, 128], BF16)
                qrope = S.sb("qrope", [16, 8, 128], BF16)
                Dg = S.sb("Dg", [128, 8, 128], BF16)
                R = [S.sb("R%d" % i, [128, 2, 512], BF16) for i in range(2)]
                score = S.sb("score", [128, SEQ], F32)
                junk = S.sb("junk", [128, SEQ], BF16)
                mask = S.sb("mask", [128, SEQ], BF16)
                maskT = S.sb("maskT", [128, 64, 128], BF16)
                st8 = S.sb("st8", [128, 8], F32)
                WH = S.sb("WH", [128, NIT], F32)
                cnt = S.sb("cnt", [128, NIT], F32)
                PT = [S.sb("PT%d" % i, [128, 8, 128], BF16) for i in range(2)]
                rD = S.sb("rD", [128, 1024], F32)
                olat = S.sb("olat", [128, 8, 128], BF16)
                oaT_all = S.sb("oaT_all", [128, 4, 2048], BF16)
                op('dve', lambda e: e.memset(oaT_all[:], 0.0), writes=['oaT_all'])

                for i in range(4 * not_b):
                    ng = i + 1
                    nkc = 4 * ng
                    n = 512 * ng
                    dma('sp', qidx[:].rearrange("p h q -> p (h q)"), qidx_d[i], reads=['qidx_d'], writes=['qidx'])
                    dma('sp', qlat[:].rearrange("p h q -> p (h q)"), qlat_d[i], reads=['qlat_d'], writes=['qlat'])
                    dma('sp', qrope[:].rearrange("p h q -> p (h q)"), qrope_d[i], reads=['qrope_d'], writes=['qrope'])
                    for h in range(8):
                        op('dve', lambda e, h=h, i=i: e.tensor_scalar(Dg[:, h, :], identb[:], widx_all[:, i, h:h + 1], None, ALU.mult),
                           reads=['identb', 'widx_all'], writes=['Dg'])
                    for g in range(ng):
                        ks = slice(g * 512, (g + 1) * 512)
                        bS = 4 + g % 2
                        for hp in range(4):
                            b0 = 2 * (hp % 2)
                            Rn = 'R%d' % (hp % 2)
                            Rt = R[hp % 2]
                            for hh in range(2):
                                op('pe', lambda e, hh=hh, hp=hp, b0=b0, ks=ks: e.matmul(
                                    ps[:, b0 + hh, :], qidx[:, 2 * hp + hh, :], kidx_s[:, ks], start=True, stop=True),
                                   reads=['qidx', 'kidx_s'], writes=[P(b0 + hh)])
                            for hh in range(2):
                                op('act', lambda e, Rt=Rt, b0=b0, hh=hh: e.activation(Rt[:, hh, :], ps[:, b0 + hh, :], AF.Relu),
                                   reads=[P(b0 + hh)], writes=[Rn])
                            for hh in range(2):
                                op('pe', lambda e, hh=hh, hp=hp, Rt=Rt, bS=bS: e.matmul(
                                    ps[:, bS, :], Dg[:, 2 * hp + hh, :], Rt[:, hh, :],
                                    start=(hp == 0 and hh == 0), stop=(hp == 3 and hh == 1)),
                                   reads=['Dg', Rn], writes=[P(bS)])
                        op('dve', lambda e, ks=ks, bS=bS: e.tensor_copy(score[:, ks], ps[:, bS, :]), reads=[P(bS)], writes=['score'])
                    op('dve', lambda e, n=n: e.tensor_reduce(st8[:, 0:1], score[:, 0:n], AX.X, ALU.max, apply_absolute_value=True),
                       reads=['score'], writes=['st8'])
                    op('dve', lambda e, n=n: e.tensor_tensor(score[:, n - 512:n], score[:, n - 512:n], cneg[:], ALU.add),
                       reads=['score', 'cneg'], writes=['score'])
                    op('dve', lambda e: e.tensor_scalar(st8[:, 1:2], st8[:, 0:1], -1.0, -1.0, ALU.mult, ALU.add), reads=['st8'], writes=['st8'])
                    op('dve', lambda e: e.tensor_scalar(st8[:, 2:3], st8[:, 0:1], 2.0, 1.0, ALU.mult, ALU.add), reads=['st8'], writes=['st8'])
                    op('dve', lambda e: e.tensor_scalar(WH[:], pw[:], st8[:, 2:3], None, ALU.mult), reads=['pw', 'st8'], writes=['WH'])
                    op('dve', lambda e: e.memset(cnt[:], 0.0), writes=['cnt'])
                    for k in range(NIT):
                        op('dve', lambda e, k=k: e.tensor_tensor(st8[:, 3:4], st8[:, 1:2], WH[:, k:k + 1], ALU.add),
                           reads=['st8', 'WH'], writes=['st8'])
                        op('dve', lambda e, k=k, n=n: e.tensor_scalar(junk[:, 0:n], score[:, 0:n], st8[:, 3:4], 0.0, ALU.is_gt, ALU.add,
                                                                      accum_out=cnt[:, k:k + 1]),
                           reads=['score', 'st8', 'cnt'], writes=['junk', 'cnt'])
                        op('dve', lambda e, k=k: e.scalar_tensor_tensor(st8[:, 4:5], cnt[:, k:k + 1], 255.5, WH[:, k:k + 1],
                                                                        ALU.is_ge, ALU.mult), reads=['cnt', 'WH', 'st8'], writes=['st8'])
                        op('dve', lambda e: e.tensor_tensor(st8[:, 1:2], st8[:, 1:2], st8[:, 4:5], ALU.add), reads=['st8'], writes=['st8'])
                    op('dve', lambda e, n=n: e.tensor_scalar(mask[:, 0:n], score[:, 0:n], st8[:, 1:2], None, ALU.is_gt),
                       reads=['score', 'st8'], writes=['mask'])
                    for c4 in range(ng):
                        b = 6 + c4 % 2
                        for cc in range(4):
                            c = 4 * c4 + cc
                            op('pe', lambda e, c=c, cc=cc, b=b: e.matmul(ps[:, b, cc * 128:(cc + 1) * 128], mask[:, c * 128:(c + 1) * 128],
                                                                         identb[:], start=True, stop=True),
                               reads=['mask', 'identb'], writes=[P(b)])
                        op('act', lambda e, c4=c4, b=b: e.copy(maskT[:, 4 * c4:4 * c4 + 4, :].rearrange("p a b -> p (a b)"), ps[:, b, :]),
                           reads=[P(b)], writes=['maskT'])
                    for c in range(nkc):
                        kc_ = slice(c * 128, (c + 1) * 128)
                        st = 2 * (c % 2)
                        Pt = PT[c % 2]
                        Pn = 'PT%d' % (c % 2)
                        for hf in range(2):
                            op('pe', lambda e, hf=hf, st=st, kc_=kc_: e.matmul(
                                ps[:, st + hf, :], ckvT_s[:, kc_], qlat[:, 4 * hf:4 * hf + 4, :].rearrange("p h q -> p (h q)"),
                                start=True, stop=False), reads=['ckvT_s', 'qlat'], writes=[P(st + hf)])
                            op('pe', lambda e, hf=hf, st=st, kc_=kc_: e.matmul(
                                ps[:, st + hf, :], krope_s[:, kc_], qrope[:, 4 * hf:4 * hf + 4, :].rearrange("p h q -> p (h q)"),
                                start=False, stop=True), reads=['krope_s', 'qrope'], writes=[P(st + hf)])
                        for hf in range(2):
                            op('act', lambda e, Pt=Pt, st=st, hf=hf: e.activation(Pt[:, 4 * hf:4 * hf + 4, :].rearrange("p h q -> p (h q)"),
                                                                                 ps[:, st + hf, :], AF.Exp, scale=0.125),
                               reads=[P(st + hf)], writes=[Pn])
                        op('dve', lambda e, Pt=Pt, c=c: e.tensor_tensor(Pt[:], Pt[:], maskT[:, c:c + 1, :].to_broadcast([128, 8, 128]), ALU.mult),
                           reads=[Pn, 'maskT'], writes=[Pn])
                        for hf in range(2):
                            rhs = Pt[:, 4 * hf:4 * hf + 4, :].rearrange("p h q -> p (h q)")
                            op('pe', lambda e, hf=hf, c=c, rhs=rhs: e.matmul(ps[:, 4 + hf, :], ckvtok_s[:, c, :], rhs,
                                                                           start=(c == 0), stop=(c == nkc - 1)),
                               reads=['ckvtok_s', Pn], writes=[P(4 + hf)])
                            op('pe', lambda e, hf=hf, c=c, rhs=rhs: e.matmul(ps[:, 6 + hf, :], onesb[:], rhs,
                                                                           start=(c == 0), stop=(c == nkc - 1)),
                               reads=['onesb', Pn], writes=[P(6 + hf)])
                    for hf in range(2):
                        op('act', lambda e, hf=hf: e.activation(rD[:, hf * 512:(hf + 1) * 512], ps[:, 6 + hf, :], AF.Ln), reads=[P(6 + hf)], writes=['rD'])
                        op('act', lambda e, hf=hf: e.activation(rD[:, hf * 512:(hf + 1) * 512], rD[:, hf * 512:(hf + 1) * 512], AF.Exp, scale=-1.0),
                           reads=['rD'], writes=['rD'])
                        op('dve', lambda e, hf=hf: e.tensor_tensor(olat[:, 4 * hf:4 * hf + 4, :].rearrange("p h q -> p (h q)"), ps[:, 4 + hf, :],
                                                                   rD[:, hf * 512:(hf + 1) * 512], ALU.mult), reads=[P(4 + hf), 'rD'], writes=['olat'])
                    b = 0
                    for p_ in range(4):
                        for hh in range(2):
                            op('pe', lambda e, p_=p_, hh=hh: e.matmul(ps[:, b, p_ * 128:(p_ + 1) * 128], wuvpad[:, 2 * p_ + hh, :],
                                                                      olat[:, 2 * p_ + hh, :], start=(hh == 0), stop=(hh == 1)),
                               reads=['wuvpad', 'olat'], writes=[P(b)])
                    op('act', lambda e, i=i: e.copy(oaT_all[:, :, i * 128:(i + 1) * 128], ps[:, b, :].rearrange("p (a q) -> p a q", q=128)),
                       reads=[P(b)], writes=['oaT_all'])
                dma('sp', oaT_d, oaT_all[:].rearrange("p a t -> p (a t)"), reads=['oaT_all'], writes=['oaT_d'])

        if stop_after >= 4:
            with S.phase():
                kb_s = S.sb("kb_s", [64, 4, SEQ], BF16)
                v_s = S.sb("v_s", [128, 64, 4, 80], BF16)
                cmT = S.sb("cmT", [128, 4, 128], BF16)
                qbb = S.sb("qbb", [64, 4, 128], BF16)
                qbf = S.sb("qbf", [64, 4, 128], F32)
                gm = S.sb("gm", [128, 4, 32], F32)
                sel = S.sb("sel", [128, 4, 32], F32)
                top8 = S.sb("top8", [128, 4, 8], F32)
                acc = S.sb("acc", [128, 4, 65], F32)
                tmpo = [S.sb("tmpo%d" % i, [128, 4, 65], F32) for i in range(2)]
                PTm = [S.sb("PTm%d" % i, [128, 4, 128], BF16) for i in range(4)]
                rden = S.sb("rden", [128, 4, 1], F32)
                ob = S.sb("ob", [128, 4, 64], BF16)
                obT_all = S.sb("obT_all", [128, 4, 2048], BF16)
                op('dve', lambda e: e.memset(obT_all[:], 0.0), writes=['obT_all'])
                dma('pool', cmT[:].rearrange("p a b -> p (a b)"), cmTd, writes=['cmT'])
                for hf in range(2):
                    for hh in range(4):
                        for q2 in range(2):
                            cs = slice(q2 * 4096, (q2 + 1) * 4096)
                            dma('sp', kb_s[:, hh, cs], kbT_d[4 * hf + hh, :, cs], reads=['kbT_d'], writes=['kb_s'])
                    for q8 in range(8):
                        dma('sp', v_s[:, 8 * q8:8 * q8 + 8, :, :].rearrange("p c h e -> p c (h e)"),
                            vaug_d[hf, 8 * q8:8 * q8 + 8].rearrange("c p x -> p c x"), reads=['vaug_d'], writes=['v_s'])
                    for i in range(4 * not_b):
                        hs = slice(hf * 512, (hf + 1) * 512)
                        dma('sp', qbb[:].rearrange("p h q -> p (h q)"), qbb_d[i, :, hs], reads=['qbb_d'], writes=['qbb'])
                        dma('sp', qbf[:].rearrange("p h q -> p (h q)"), qbf_d[i, :, hs], reads=['qbf_d'], writes=['qbf'])
                        bg = nbank()
                        for hh in range(4):
                            op('pe', lambda e, hh=hh, bg=bg: e.matmul(ps[:, bg, hh * 32:(hh + 1) * 32], qbf[:, hh, :], kmean[:, 4 * hf + hh, :],
                                                                      start=True, stop=True), reads=['qbf', 'kmean'], writes=[P(bg)])
                        op('dve', lambda e, bg=bg: e.tensor_copy(gm[:].rearrange("p h n -> p (h n)"), ps[:, bg, 0:128]), reads=[P(bg)], writes=['gm'])
                        op('dve', lambda e, i=i: e.tensor_scalar(gm[:, :, 2 * i:2 * i + 1], gm[:, :, 2 * i:2 * i + 1], flag_s[:, 1:2], None, ALU.add),
                           reads=['gm', 'flag_s'], writes=['gm'])
                        op('dve', lambda e, i=i: e.memset(gm[:, :, 2 * i + 1:32], -BIG), reads=['gm'], writes=['gm'])
                        for hh in range(4):
                            op('dve', lambda e, hh=hh: e.max(top8[:, hh, :], gm[:, hh, :]), reads=['gm'], writes=['top8'])
                        for hh in range(4):
                            op('dve', lambda e, hh=hh: e.tensor_scalar(sel[:, hh, :], gm[:, hh, :], top8[:, hh, 2:3], None, ALU.is_ge),
                               reads=['gm', 'top8'], writes=['sel'])
                        op('dve', lambda e, i=i: e.tensor_scalar(sel[:, :, 2 * i:2 * i + 1], sel[:, :, 2 * i:2 * i + 1], flag_s[:, 0:1], None, ALU.max),
                           reads=['sel', 'flag_s'], writes=['sel'])
                        op('dve', lambda e, i=i: e.memset(sel[:, :, 2 * i + 1:2 * i + 2], 1.0), reads=['sel'], writes=['sel'])
                        op('dve', lambda e: e.memset(acc[:], 0.0), writes=['acc'])
                        for nb_ in range(2 * i + 2):
                            bO = 2 + nb_ % 2
                            pov = ps[:, bO, :].rearrange("p (h x) -> p h x", x=128)
                            for cc in range(2):
                                c = 2 * nb_ + cc
                                bST = c % 2
                                Pt = PTm[c % 4]
                                Pn = 'PTm%d' % (c % 4)
                                for hh in range(4):
                                    op('pe', lambda e, hh=hh, c=c, bST=bST: e.matmul(
                                        ps[:, bST, hh * 128:(hh + 1) * 128], kb_s[:, hh, c * 128:(c + 1) * 128], qbb[:, hh, :],
                                        start=True, stop=True), reads=['kb_s', 'qbb'], writes=[P(bST)])
                                op('act', lambda e, Pt=Pt, bST=bST: e.activation(Pt[:].rearrange("p h q -> p (h q)"), ps[:, bST, :], AF.Exp, scale=0.125),
                                   reads=[P(bST)], writes=[Pn])
                                if nb_ >= 2 * i:
                                    cl = c - 4 * i
                                    op('dve', lambda e, Pt=Pt, cl=cl: e.tensor_tensor(Pt[:], Pt[:], cmT[:, cl:cl + 1, :].to_broadcast([128, 4, 128]), ALU.mult),
                                       reads=[Pn, 'cmT'], writes=[Pn])
                            for hh in range(4):
                                for cc in range(2):
                                    c = 2 * nb_ + cc
                                    Pt = PTm[c % 4]
                                    Pn = 'PTm%d' % (c % 4)
                                    op('pe', lambda e, hh=hh, c=c, cc=cc, Pt=Pt, pov=pov: e.matmul(
                                        pov[:, hh, 0:65], Pt[:, hh, :], v_s[:, c, hh, 0:65], start=(cc == 0), stop=(cc == 1)),
                                       reads=[Pn, 'v_s'], writes=[P(bO)])
                            to = tmpo[nb_ % 2]
                            tn = 'tmpo%d' % (nb_ % 2)
                            op('dve', lambda e, to=to, pov=pov, nb_=nb_: e.tensor_tensor(
                                to[:], pov[:, :, 0:65], sel[:, :, nb_:nb_ + 1].to_broadcast([128, 4, 65]), ALU.mult),
                               reads=[P(bO), 'sel'], writes=[tn])
                            op('dve', lambda e, to=to: e.tensor_tensor(acc[:], acc[:], to[:], ALU.add), reads=['acc', tn], writes=['acc'])
                        op('act', lambda e: e.activation(rden[:], acc[:, :, 64:65], AF.Ln), reads=['acc'], writes=['rden'])
                        op('act', lambda e: e.activation(rden[:], rden[:], AF.Exp, scale=-1.0), reads=['rden'], writes=['rden'])
                        op('dve', lambda e: e.tensor_tensor(ob[:], acc[:, :, 0:64], rden[:].to_broadcast([128, 4, 64]), ALU.mult),
                           reads=['acc', 'rden'], writes=['ob'])
                        bt = nbank()
                        obv = ob[:].rearrange("p h d -> p (h d)")
                        for j2 in range(2):
                            op('pe', lambda e, j2=j2, bt=bt, obv=obv: e.matmul(ps[:, bt, j2 * 128:(j2 + 1) * 128], obv[:, j2 * 128:(j2 + 1) * 128],
                                                                              identb[:], start=True, stop=True),
                               reads=['ob', 'identb'], writes=[P(bt)])
                        op('act', lambda e, i=i, bt=bt, hf=hf: e.copy(obT_all[:, 2 * hf:2 * hf + 2, i * 128:(i + 1) * 128],
                                                                     ps[:, bt, 0:256].rearrange("p (a q) -> p a q", q=128)),
                           reads=[P(bt)], writes=['obT_all'])
                dma('sp', obT_d, obT_all[:].rearrange("p a t -> p (a t)"), reads=['obT_all'], writes=['obT_d'])

        if stop_after >= 5:
            with S.phase():
                wg = S.sb("wg", [128, 8, 2048], BF16)
                wba = S.sb("wba", [128, 4, D], BF16)
                wbb = S.sb("wbb", [128, 4, D], BF16)
                wo_ = S.sb("wo_", [128, 8, D], BF16)
                wgv = wgd.rearrange("(k p) c -> p k c", p=128)
                wov = wod.rearrange("(k p) c -> p k c", p=128)
                for k in range(8):
                    dma('pool', wg[:, k, :], wgv[:, k, :], writes=['wg'])
                    dma('pool', wo_[:, k, :], wov[:, k, :], writes=['wo_'])
                wbav = wbad.rearrange("(k p) c -> p k c", p=128)
                wbbv = wbbd.rearrange("(k p) c -> p k c", p=128)
                for k in range(4):
                    dma('pool', wba[:, k, :], wbav[:, k, :], writes=['wba'])
                    dma('pool', wbb[:, k, :], wbbv[:, k, :], writes=['wbb'])
                oaT_all = S.sb("oaT_all", [128, 4, 2048], BF16)
                obT_all = S.sb("obT_all", [128, 4, 2048], BF16)
                dma('sp', oaT_all[:].rearrange("p a t -> p (a t)"), oaT_d, reads=['oaT_d'], writes=['oaT_all'])
                dma('sp', obT_all[:].rearrange("p a t -> p (a t)"), obT_d, reads=['obT_d'], writes=['obT_all'])
                h2o = S.sb("h2o", [128, 8, 512], BF16)
                x1o = S.sb("x1o", [128, 8, 512], F32)
                x2 = S.sb("x2", [128, 8, 512], F32)
                yT = S.sb("yT", [128, 8, 512], BF16)
                sg = [S.sb("sg%d" % i, [128, 512], F32) for i in range(2)]
                tA = S.sb("tA", [128, 512], F32)
                tB = S.sb("tB", [128, 512], F32)
                h2ov = h2oT_d.rearrange("(k p) t -> p k t", p=128)
                x1ov = x1oT_d.rearrange("(k p) t -> p k t", p=128)
                x2ov = x2oT_d.rearrange("(k p) t -> p k t", p=128)
                for ot in range(not_b):
                    cols = slice(ot * 512, (ot + 1) * 512)
                    dma('sp', h2o[:], h2ov[:, :, cols], reads=['h2o_d'], writes=['h2o'])
                    dma('sp', x1o[:], x1ov[:, :, cols], reads=['x1o_d'], writes=['x1o'])
                    for d in range(8):
                        for side, (wbr, oT, tt_, tn) in enumerate(((wba, oaT_all, tA, 'tA'), (wbb, obT_all, tB, 'tB'))):
                            bG = nbank()
                            for k in range(8):
                                op('pe', lambda e, k=k, d=d, side=side, bG=bG: e.matmul(
                                    ps[:, bG, :], wg[:, k, side * 1024 + d * 128:side * 1024 + (d + 1) * 128], h2o[:, k, :],
                                    start=(k == 0), stop=(k == 7)), reads=['wg', 'h2o'], writes=[P(bG)])
                            op('act', lambda e, side=side, bG=bG: e.activation(sg[side][:], ps[:, bG, :], AF.Sigmoid),
                               reads=[P(bG)], writes=['sg%d' % side])
                            bA = nbank()
                            for k in range(4):
                                op('pe', lambda e, k=k, d=d, bA=bA, wbr=wbr, oT=oT: e.matmul(
                                    ps[:, bA, :], wbr[:, k, d * 128:(d + 1) * 128], oT[:, k, cols], start=(k == 0), stop=(k == 3)),
                                   reads=['wba', 'wbb', 'oaT_all', 'obT_all'], writes=[P(bA)])
                            op('dve', lambda e, side=side, bA=bA, tt_=tt_: e.tensor_tensor(tt_[:], sg[side][:], ps[:, bA, :], ALU.mult),
                               reads=['sg%d' % side, P(bA)], writes=[tn])
                        op('dve', lambda e, d=d: e.tensor_tensor(yT[:, d, :], tA[:], tB[:], ALU.add), reads=['tA', 'tB'], writes=['yT'])
                    for d2 in range(8):
                        bY = nbank()
                        for k in range(8):
                            op('pe', lambda e, k=k, d2=d2, bY=bY: e.matmul(ps[:, bY, :], wo_[:, k, d2 * 128:(d2 + 1) * 128], yT[:, k, :],
                                                                          start=(k == 0), stop=(k == 7)), reads=['wo_', 'yT'], writes=[P(bY)])
                        op('dve', lambda e, d2=d2, bY=bY: e.scalar_tensor_tensor(x2[:, d2, :], ps[:, bY, :], cst[:, 5, d2:d2 + 1], x1o[:, d2, :],
                                                                                ALU.mult, ALU.add), reads=[P(bY), 'x1o', 'cst'], writes=['x2'])
                    dma('sp', x2ov[:, :, cols], x2[:], reads=['x2'], writes=['x2o_d'])

        if stop_after >= 6:
            with S.phase():
                wi = S.sb("wi", [128, 8, 2 * DFF], BF16)
                wo = S.sb("wo", [128, NF, D], BF16)
                load_ffn_w(w2i, w2o, wi, wo)
                W = ffn_work()
                xb = [S.sb("xb0", [128, 8, 512], F32)] * 2
                of = S.sb("of", [128, 8, 512], F32)
                x2ov = x2oT_d.rearrange("(k p) t -> p k t", p=128)
                outv = outT.rearrange("(k p) t -> p k t", p=128)
                for ot in range(not_b):
                    cols = slice(ot * 512, (ot + 1) * 512)
                    xt = xb[0]
                    xn = 'xb0'
                    dma('sp', xt[:], x2ov[:, :, cols], reads=['x2o_d'], writes=[xn])
                    ffn_tile(xt, xn, cst[:, 6, :], cst[:, 7, :], cst[:, 8, :], wi, wo, xt, xn, W)
                    norm_mod(xt, xn, gfin, None, of, 'of', W['sq'], W['rp'], W['tmp'])
                    dma('sp', outv[:, :, cols], of[:], reads=['of'], writes=['outT'])
        S.barrier()
    return nc


def _prep_inputs(x, c, positions, ada_w, ada_b, norm1_g, ffn1_w_in, ffn1_w_out, norm2_g, w_in, kv_norm_g, w_uk, w_uv,
                 w_branch_a, w_branch_b, w_out, norm3_g, ffn2_w_in, ffn2_w_out, final_g):
    f32 = np.float32
    x = np.asarray(x, f32)
    c = np.asarray(c, f32)
    positions = np.asarray(positions, np.int32)
    w_in = np.asarray(w_in, f32)[0]
    o_qa, o_ckv, o_kr, o_qi, o_ki, o_wi, o_qb, o_kb, o_vb, o_ga, o_gb = np.cumsum(
        [0, 512, 128, 16, 512, 64, 8, 512, 512, 512, 1024]).tolist()
    qa = w_in[:, o_qa:o_qa + 512].reshape(D, 8, 64)
    qidx = w_in[:, o_qi:o_qi + 512].reshape(D, 8, 64)
    qb = w_in[:, o_qb:o_qb + 512].reshape(D, 8, 64)
    kb = w_in[:, o_kb:o_kb + 512].reshape(D, 8, 64)
    wk = np.concatenate([w_in[:, o_ckv:o_ckv + 128], w_in[:, o_kr:o_kr + 16], w_in[:, o_ki:o_ki + 64],
                         w_in[:, o_kb:o_kb + 512], w_in[:, o_vb:o_vb + 512]], axis=1)
    wkr = np.concatenate([w_in[:, o_kr:o_kr + 16], w_in[:, o_ki:o_ki + 16], kb[:, :, :16].reshape(D, 128)], axis=1)
    wq = np.concatenate([qa[:, :, 16:].reshape(D, 384), qa[:, :, :16].reshape(D, 128), qidx.reshape(D, 512),
                         w_in[:, o_wi:o_wi + 8], qb.reshape(D, 512)], axis=1)
    wqr = np.concatenate([qa[:, :, :16].reshape(D, 128), qidx[:, :, :16].reshape(D, 128), qb[:, :, :16].reshape(D, 128)], axis=1)
    wg = w_in[:, o_ga:o_ga + 2048]
    assert wk.shape[1] == KC and wkr.shape[1] == KRC and wq.shape[1] == QC and wqr.shape[1] == QRC
    half = 8
    invf = np.power(np.float32(500000.0), -np.arange(half, dtype=f32) / half).astype(f32)
    invf16 = np.concatenate([invf, invf]).reshape(16, 1)
    pw = np.tile((2.0 ** -(np.arange(NIT, dtype=np.float64) + 1)).astype(f32)[None, :], (128, 1))
    gn = np.stack([np.asarray(g, f32).reshape(8, 128).T for g in (norm1_g[0], norm2_g[0], norm3_g[0], final_g)], axis=1)
    shared = {
        "invf": invf16, "identd": np.eye(128, dtype=f32), "pwd": pw,
        "ada_w": np.ascontiguousarray(np.asarray(ada_w, f32)[0]),
        "ada_bT": np.ascontiguousarray(np.asarray(ada_b, f32)[0].reshape(72, 128).T),
        "gnd": np.ascontiguousarray(gn.reshape(128, 32)),
        "gkvd": np.ascontiguousarray(np.asarray(kv_norm_g, f32)[0].reshape(128, 1)),
        "w1i": np.ascontiguousarray(np.asarray(ffn1_w_in, f32)[0]), "w1o": np.ascontiguousarray(np.asarray(ffn1_w_out, f32)[0]),
        "w2i": np.ascontiguousarray(np.asarray(ffn2_w_in, f32)[0]), "w2o": np.ascontiguousarray(np.asarray(ffn2_w_out, f32)[0]),
        "wkd": np.ascontiguousarray(wk), "wkrd": np.ascontiguousarray(wkr), "wqd": np.ascontiguousarray(wq),
        "wqrd": np.ascontiguousarray(wqr), "wgd": np.ascontiguousarray(wg),
        "wukTd": np.ascontiguousarray(np.asarray(w_uk, f32)[0].transpose(2, 1, 0).reshape(48, 1024)),
        "wuvd": np.ascontiguousarray(np.asarray(w_uv, f32)[0].reshape(128, 512)),
        "wbad": np.ascontiguousarray(np.asarray(w_branch_a, f32)[0]), "wbbd": np.ascontiguousarray(np.asarray(w_branch_b, f32)[0]),
        "wod": np.ascontiguousarray(np.asarray(w_out, f32)[0]),
    }
    in_maps = []
    for core in range(8):
        b, j = core // 4, core % 4
        xTb = np.ascontiguousarray(x[b].T)
        own = np.concatenate([np.arange((4 * i + j) * 128, (4 * i + j + 1) * 128) for i in range(16)])
        ql = np.arange(128)[:, None]
        kl = np.arange(512)[None, :]
        allowed = kl <= (j * 128 + ql)
        cneg = np.where(allowed, 0.0, -BIG).astype(f32)
        cmT = np.ascontiguousarray(allowed.T.reshape(4, 128, 128).transpose(1, 0, 2).reshape(128, 512)).astype(f32)
        ownlo = 1.0 if j < 2 else 0.0
        flag = np.tile(np.array([[ownlo, -BIG * ownlo]], dtype=f32), (128, 1))
        m = dict(shared)
        m.update({
            "xT": xTb, "xoT": np.ascontiguousarray(xTb[:, own]),
            "cT": np.ascontiguousarray(c[b].reshape(8, 128).T),
            "posb": np.ascontiguousarray(positions[b:b + 1]), "poso": np.ascontiguousarray(positions[b:b + 1, own]),
            "cnegd": cneg, "cmTd": cmT, "flagd": flag,
        })
        in_maps.append(m)
    return in_maps


def kernel(**inputs):
    in_maps = _prep_inputs(**inputs)
    import os
    nc = build_nc(stop_after=int(os.environ.get('KSTOP', '99')))
    res = run_bass_kernel_spmd(nc, in_maps, core_ids=list(range(8)))
    out = np.empty((2, SEQ, D), np.float32)
    for core in range(8):
        b, j = core // 4, core % 4
        oT = np.asarray(res.results[core]["outT"])
        for i in range(16):
            G = 4 * i + j
            out[b, G * 128:(G + 1) * 128, :] = oT[:, i * 128:(i + 1) * 128].T
    return out
```
